# Optimizing a Trainium2 kernel written in Bass

```python
import jax, jax.numpy as jnp
from jax import lax
import numpy as np


D_MODEL = 2048
BATCH = 4
SEQ = 2048
DEPTH = 4

RET_HEADS = 4
RET_DK = 64
RET_DV = 128
RET_CHUNK = 128
ROPE_BASE = 10000.0
SGU_GROUPS = 4
SGU_DIM = 128
SGU_CHUNK = 128
NSA_HEADS = 16
NSA_GROUPS = 4
NSA_HPG = NSA_HEADS // NSA_GROUPS
NSA_DK = 64
CMP_LEN = 32
CMP_STRIDE = 16
CMP_HIDDEN = 256
SEL_LEN = 64
SEL_TOPN = 8
WINDOW = 512
Q_BLOCK = 128
PEER_HEADS = 8
PEER_NKEYS = 128
PEER_EXPERTS = PEER_NKEYS * PEER_NKEYS
PEER_QDIM = 256
PEER_HALF = PEER_QDIM // 2
PEER_TOPK = 16
PEER_CHUNK = 128

RET_W = RET_HEADS * RET_DV
SGU_W = SGU_GROUPS * SGU_DIM
NSA_W = NSA_HEADS * NSA_DK
NSA_KV = NSA_GROUPS * NSA_DK
MIX_W = RET_W + SGU_W + NSA_W
IN_WIDTHS = (RET_HEADS * RET_DK, RET_HEADS * RET_DK, RET_W, RET_W, SGU_W, SGU_W, NSA_W, NSA_KV, NSA_KV, NSA_KV, NSA_KV, NSA_KV, NSA_KV, NSA_HEADS * 3)
IN_W = sum(IN_WIDTHS)

DEEPNORM_ALPHA = (2 * DEPTH) ** 0.25
DEEPNORM_BETA = (8 * DEPTH) ** -0.25
LN_EPS = 1e-5
NEG_INF = -1e9
FORCE_SCORE = 1e6

kernel_name = 'hybrid_retention_gmlp_nsa_peer_deepnorm'


def _layer_norm(x, g, b):
    xf = x.astype(jnp.float32)
    mu = jnp.mean(xf, -1, keepdims=True)
    var = jnp.mean(jnp.square(xf - mu), -1, keepdims=True)
    return (xf - mu) * lax.rsqrt(var + LN_EPS) * g + b


def _rms_normalize(y):
    yf = y.astype(jnp.float32)
    return yf * lax.rsqrt(jnp.mean(jnp.square(yf), -1, keepdims=True) + LN_EPS)


def _rotate(x, pos):
    half = x.shape[-1] // 2
    inv = ROPE_BASE ** (-jnp.arange(half, dtype=jnp.float32) / half)
    ang = pos.astype(jnp.float32)[:, None] * inv[None, :]
    cos = jnp.cos(ang)[None, :, None, :]
    sin = jnp.sin(ang)[None, :, None, :]
    x1, x2 = x[..., :half], x[..., half:]
    return jnp.concatenate([x1 * cos - x2 * sin, x1 * sin + x2 * cos], -1)


def _retention(q, k, v, g, gn_g, gn_b):
    B, T = q.shape[:2]
    pos = jnp.arange(T)
    q = _rotate(q.astype(jnp.float32), pos)
    k = _rotate(k.astype(jnp.float32), pos) * (RET_DK ** -0.5)
    v = v.astype(jnp.float32)
    gamma = 1.0 - 2.0 ** (-5.0 - jnp.arange(RET_HEADS, dtype=jnp.float32))
    lg = jnp.log(gamma)
    n = jnp.arange(RET_CHUNK, dtype=jnp.float32)
    diff = n[:, None] - n[None, :]
    d_in = jnp.where(diff[None] >= 0, jnp.exp(lg[:, None, None] * jnp.maximum(diff, 0.0)[None]), 0.0)
    xi = jnp.exp(lg[:, None] * (n[None, :] + 1.0))
    zeta = jnp.exp(lg[:, None] * (RET_CHUNK - 1.0 - n[None, :]))
    g_chunk = jnp.exp(lg * RET_CHUNK)
    nc = T // RET_CHUNK

    def to_chunks(a):
        return a.reshape(B, nc, RET_CHUNK, RET_HEADS, a.shape[-1]).transpose(1, 0, 3, 2, 4)

    def step(state, inp):
        qi, ki, vi = inp
        att = jnp.einsum('bhnd,bhmd->bhnm', qi, ki) * d_in[None]
        inner = jnp.einsum('bhnm,bhme->bhne', att, vi)
        cross = jnp.einsum('bhnd,bhde->bhne', qi, state) * xi[None, :, :, None]
        state = state * g_chunk[None, :, None, None] + jnp.einsum('bhmd,bhme->bhde', ki * zeta[None, :, :, None], vi)
        return state, inner + cross

    s0 = jnp.zeros((B, RET_HEADS, RET_DK, RET_DV), jnp.float32)
    _, y = lax.scan(step, s0, (to_chunks(q), to_chunks(k), to_chunks(v)))
    y = y.transpose(1, 0, 3, 2, 4).reshape(B, T, RET_HEADS, RET_DV)
    mu = jnp.mean(y, -1, keepdims=True)
    var = jnp.mean(jnp.square(y - mu), -1, keepdims=True)
    y = ((y - mu) * lax.rsqrt(var + LN_EPS)).reshape(B, T, RET_W) * gn_g + gn_b
    return jax.nn.silu(g.astype(jnp.float32)) * y


def _spatial_gating(u, v, ln_g, ln_b, w_s, b_s):
    B, T = u.shape[:2]
    nc = T // SGU_CHUNK
    v = _layer_norm(v.reshape(B, T, SGU_GROUPS, SGU_DIM), ln_g.reshape(SGU_GROUPS, SGU_DIM), ln_b.reshape(SGU_GROUPS, SGU_DIM))
    v = v.reshape(B, nc, SGU_CHUNK, SGU_GROUPS, SGU_DIM)
    causal = jnp.tril(jnp.ones((SGU_CHUNK, SGU_CHUNK), jnp.float32))
    w = w_s.astype(jnp.float32) * causal[None]
    s = jnp.einsum('gts,bcsgd->bctgd', w, v) + b_s.T.astype(jnp.float32)[None, None, :, :, None]
    return u.astype(jnp.float32) * s.reshape(B, T, SGU_W)


def _compress(k, pe, w1, w2):
    B, G, T, dk = k.shape
    nseg = T // CMP_STRIDE
    r = CMP_LEN // CMP_STRIDE
    nc = nseg - r + 1
    seg = k.reshape(B, G, nseg, CMP_STRIDE, dk)
    blk = jnp.concatenate([seg[:, :, j:j + nc] for j in range(r)], axis=3) + pe
    hid = jax.nn.gelu(blk.reshape(B, G, nc, CMP_LEN * dk) @ w1)
    return hid @ w2


def _nsa(q, k_cmp, v_cmp, k_slc, v_slc, k_win, v_win, gate, pe_k, w1_k, w2_k, pe_v, w1_v, w2_v):
    B, T = q.shape[:2]
    G, H, dk = NSA_GROUPS, NSA_HPG, NSA_DK
    scale = dk ** -0.5
    qh = q.reshape(B, T, G, H, dk).transpose(0, 2, 3, 1, 4)

    def kv(a):
        return a.reshape(B, T, G, dk).transpose(0, 2, 1, 3)

    t = jnp.arange(T)

    kc = _compress(kv(k_cmp), pe_k, w1_k, w2_k)
    vc = _compress(kv(v_cmp), pe_v, w1_v, w2_v)
    ncmp = kc.shape[2]
    cmp_start = jnp.arange(ncmp) * CMP_STRIDE
    cmask = (cmp_start[None, :] + CMP_LEN - 1) <= t[:, None]
    s = jnp.einsum('bghtd,bgcd->bghtc', qh, kc).astype(jnp.float32) * scale
    p_cmp = jnp.where(cmask, jax.nn.softmax(jnp.where(cmask, s, NEG_INF), -1), 0.0)
    o_cmp = jnp.einsum('bghtc,bgcd->bghtd', p_cmp, vc.astype(jnp.float32))

    nsel = T // SEL_LEN
    topn = min(SEL_TOPN, nsel)
    sel_start = jnp.arange(nsel) * SEL_LEN
    overlap = ((cmp_start[:, None] < sel_start[None, :] + SEL_LEN) & (cmp_start[:, None] + CMP_LEN > sel_start[None, :])).astype(jnp.float32)
    imp = jnp.einsum('bghtc,cj->bgtj', p_cmp, overlap)
    jj = jnp.arange(nsel)[None, :]
    cur = (t // SEL_LEN)[:, None]
    forced = (jj == 0) | (jj == cur) | (jj == cur - 1)
    causal_blk = sel_start[None, :] <= t[:, None]
    imp = jnp.where(causal_blk, jnp.where(forced, FORCE_SCORE, imp), NEG_INF)
    _, idx = lax.top_k(imp, topn)
    kb = kv(k_slc).reshape(B, G, nsel, SEL_LEN, dk)
    vb = kv(v_slc).reshape(B, G, nsel, SEL_LEN, dk)
    nq = T // Q_BLOCK
    q_blocks = qh.reshape(B, G, H, nq, Q_BLOCK, dk).transpose(3, 0, 1, 2, 4, 5)
    idx_blocks = idx.reshape(B, G, nq, Q_BLOCK, topn).transpose(2, 0, 1, 3, 4)
    t_blocks = t.reshape(nq, Q_BLOCK)
    bi = jnp.arange(B)[:, None, None, None]
    gi = jnp.arange(G)[None, :, None, None]

    def sel_block(args):
        qb, ib, tb = args
        kg = kb[bi, gi, ib]
        vg = vb[bi, gi, ib]
        sc = jnp.einsum('bghqd,bgqnld->bghqnl', qb, kg).astype(jnp.float32) * scale
        kpos = ib[..., None] * SEL_LEN + jnp.arange(SEL_LEN)
        m = (kpos <= tb[None, None, :, None, None])[:, :, None]
        sc = jnp.where(m, sc, NEG_INF)
        shp = sc.shape
        pr = jax.nn.softmax(sc.reshape(shp[0], shp[1], shp[2], shp[3], -1), -1).reshape(shp)
        return jnp.einsum('bghqnl,bgqnld->bghqd', pr, vg.astype(jnp.float32))

    o_sel = lax.map(sel_block, (q_blocks, idx_blocks, t_blocks))
    o_sel = o_sel.transpose(1, 2, 3, 0, 4, 5).reshape(B, G, H, T, dk)

    npad = WINDOW // Q_BLOCK
    nkeys = (npad + 1) * Q_BLOCK
    kp = jnp.pad(kv(k_win), ((0, 0), (0, 0), (WINDOW, 0), (0, 0))).reshape(B, G, nq + npad, Q_BLOCK, dk)
    vp = jnp.pad(kv(v_win), ((0, 0), (0, 0), (WINDOW, 0), (0, 0))).reshape(B, G, nq + npad, Q_BLOCK, dk)
    kband = jnp.concatenate([kp[:, :, j:j + nq] for j in range(npad + 1)], axis=3)
    vband = jnp.concatenate([vp[:, :, j:j + nq] for j in range(npad + 1)], axis=3)
    qw = qh.reshape(B, G, H, nq, Q_BLOCK, dk)
    sw = jnp.einsum('bghnqd,bgnkd->bghnqk', qw, kband).astype(jnp.float32) * scale
    qpos = jnp.arange(nq)[:, None] * Q_BLOCK + jnp.arange(Q_BLOCK)[None, :]
    kpos = jnp.arange(nq)[:, None] * Q_BLOCK - WINDOW + jnp.arange(nkeys)[None, :]
    dist = qpos[:, :, None] - kpos[:, None, :]
    wmask = (dist >= 0) & (dist < WINDOW) & (kpos[:, None, :] >= 0)
    pw = jax.nn.softmax(jnp.where(wmask, sw, NEG_INF), -1)
    o_win = jnp.einsum('bghnqk,bgnkd->bghnqd', pw, vband.astype(jnp.float32)).reshape(B, G, H, T, dk)

    gt = jax.nn.sigmoid(gate.astype(jnp.float32)).reshape(B, T, G, H, 3).transpose(0, 2, 3, 1, 4)
    o = gt[..., 0:1] * o_cmp + gt[..., 1:2] * o_sel + gt[..., 2:3] * o_win
    return o.transpose(0, 3, 1, 2, 4).reshape(B, T, NSA_W)


def _peer(x, wq, k1, k2, u_tab, v_tab):
    B, T, D = x.shape
    ntok = B * T
    xf = x.reshape(ntok, D)
    q = (xf @ wq).reshape(ntok, PEER_HEADS, 2, PEER_HALF)
    s1 = jnp.einsum('nhd,hkd->nhk', q[:, :, 0], k1).astype(jnp.float32)
    s2 = jnp.einsum('nhd,hkd->nhk', q[:, :, 1], k2).astype(jnp.float32)
    v1, i1 = lax.top_k(s1, PEER_TOPK)
    v2, i2 = lax.top_k(s2, PEER_TOPK)
    cand = (v1[..., :, None] + v2[..., None, :]).reshape(ntok, PEER_HEADS, PEER_TOPK * PEER_TOPK)
    sc, ci = lax.top_k(cand, PEER_TOPK)
    e = jnp.take_along_axis(i1, ci // PEER_TOPK, -1) * PEER_NKEYS + jnp.take_along_axis(i2, ci % PEER_TOPK, -1)
    gw = jax.nn.softmax(sc, -1)
    nch = ntok // PEER_CHUNK

    def chunk(args):
        xc, ec, gc = args
        ue = u_tab[ec]
        hact = jax.nn.gelu(jnp.einsum('nd,nhkd->nhk', xc, ue).astype(jnp.float32))
        return jnp.einsum('nhk,nhkd->nd', (gc * hact).astype(v_tab.dtype), v_tab[ec])

    out = lax.map(chunk, (xf.reshape(nch, PEER_CHUNK, D), e.reshape(nch, PEER_CHUNK, PEER_HEADS, PEER_TOPK), gw.reshape(nch, PEER_CHUNK, PEER_HEADS, PEER_TOPK)))
    return out.reshape(B, T, D).astype(x.dtype)


def setup_inputs(seed: int = 0) -> dict:
    key = jax.random.key(seed)
    ks = jax.random.split(key, 32)
    L = DEPTH

    def nrm(k, shape, std):
        return jax.random.normal(k, shape, jnp.float32) * std

    return {
        'x': nrm(ks[0], (BATCH, SEQ, D_MODEL), 1.0),
        'w_in': nrm(ks[1], (L, D_MODEL, IN_W), D_MODEL ** -0.5),
        'ret_gn_g': 1.0 + nrm(ks[2], (L, RET_W), 0.02),
        'ret_gn_b': nrm(ks[3], (L, RET_W), 0.02),
        'sgu_ln_g': 1.0 + nrm(ks[4], (L, SGU_W), 0.02),
        'sgu_ln_b': nrm(ks[5], (L, SGU_W), 0.02),
        'sgu_w': nrm(ks[6], (L, SGU_GROUPS, SGU_CHUNK, SGU_CHUNK), SGU_CHUNK ** -0.5),
        'sgu_b': 1.0 + nrm(ks[7], (L, SGU_GROUPS, SGU_CHUNK), 0.02),
        'cmp_pe_k': nrm(ks[8], (L, CMP_LEN, NSA_DK), 0.02),
        'cmp_w1_k': nrm(ks[9], (L, CMP_LEN * NSA_DK, CMP_HIDDEN), (CMP_LEN * NSA_DK) ** -0.5),
        'cmp_w2_k': nrm(ks[10], (L, CMP_HIDDEN, NSA_DK), CMP_HIDDEN ** -0.5),
        'cmp_pe_v': nrm(ks[11], (L, CMP_LEN, NSA_DK), 0.02),
        'cmp_w1_v': nrm(ks[12], (L, CMP_LEN * NSA_DK, CMP_HIDDEN), (CMP_LEN * NSA_DK) ** -0.5),
        'cmp_w2_v': nrm(ks[13], (L, CMP_HIDDEN, NSA_DK), CMP_HIDDEN ** -0.5),
        'mix_norm_g': 1.0 + nrm(ks[14], (L, MIX_W), 0.02),
        'w_out': nrm(ks[15], (L, MIX_W, D_MODEL), (MIX_W ** -0.5) * DEEPNORM_BETA),
        'ln1_g': 1.0 + nrm(ks[16], (L, D_MODEL), 0.02),
        'ln1_b': nrm(ks[17], (L, D_MODEL), 0.02),
        'peer_wq': nrm(ks[18], (L, D_MODEL, PEER_HEADS * PEER_QDIM), D_MODEL ** -0.5),
        'peer_k1': nrm(ks[19], (L, PEER_HEADS, PEER_NKEYS, PEER_HALF), PEER_HALF ** -0.5),
        'peer_k2': nrm(ks[20], (L, PEER_HEADS, PEER_NKEYS, PEER_HALF), PEER_HALF ** -0.5),
        'peer_u': nrm(ks[21], (L, PEER_EXPERTS, D_MODEL), D_MODEL ** -0.5),
        'peer_v': nrm(ks[22], (L, PEER_EXPERTS, D_MODEL), DEEPNORM_BETA * PEER_HEADS ** -0.5),
        'ln2_g': 1.0 + nrm(ks[23], (L, D_MODEL), 0.02),
        'ln2_b': nrm(ks[24], (L, D_MODEL), 0.02),
    }


def reference(x, w_in, ret_gn_g, ret_gn_b, sgu_ln_g, sgu_ln_b, sgu_w, sgu_b, cmp_pe_k, cmp_w1_k, cmp_w2_k, cmp_pe_v, cmp_w1_v, cmp_w2_v, mix_norm_g, w_out, ln1_g, ln1_b, peer_wq, peer_k1, peer_k2, peer_u, peer_v, ln2_g, ln2_b):
    B, T, _ = x.shape
    offsets = [int(o) for o in np.cumsum(IN_WIDTHS)[:-1]]
    for l in range(DEPTH):
        h = x @ w_in[l]
        rq, rk, rv, rg, su, sv, nq_, kc, vc, ksl, vsl, kw, vw, ng = jnp.split(h, offsets, axis=-1)
        y_ret = _retention(rq.reshape(B, T, RET_HEADS, RET_DK), rk.reshape(B, T, RET_HEADS, RET_DK), rv.reshape(B, T, RET_HEADS, RET_DV), rg, ret_gn_g[l], ret_gn_b[l])
        y_sgu = _spatial_gating(jax.nn.gelu(su), jax.nn.gelu(sv), sgu_ln_g[l], sgu_ln_b[l], sgu_w[l], sgu_b[l])
        y_nsa = _nsa(nq_, kc, vc, ksl, vsl, kw, vw, ng, cmp_pe_k[l], cmp_w1_k[l], cmp_w2_k[l], cmp_pe_v[l], cmp_w1_v[l], cmp_w2_v[l])
        mixed = jnp.concatenate([_rms_normalize(y_ret), _rms_normalize(y_sgu), _rms_normalize(y_nsa)], -1) * mix_norm_g[l]
        x = _layer_norm(DEEPNORM_ALPHA * x + mixed.astype(x.dtype) @ w_out[l], ln1_g[l], ln1_b[l]).astype(x.dtype)
        x = _layer_norm(DEEPNORM_ALPHA * x + _peer(x, peer_wq[l], peer_k1[l], peer_k2[l], peer_u[l], peer_v[l]), ln2_g[l], ln2_b[l]).astype(x.dtype)
    return x
```

```python
import numpy as np
from contextlib import ExitStack
import concourse.bass as bass
import concourse.mybir as mybir
from concourse.bass_utils import run_bass_kernel_spmd

F32 = mybir.dt.float32
BF16 = mybir.dt.bfloat16
I32 = mybir.dt.int32
U32 = mybir.dt.uint32
AF = mybir.ActivationFunctionType
ALU = mybir.AluOpType
AX = mybir.AxisListType

D_MODEL = 2048
BATCH = 4
SEQ = 2048
DEPTH = 4
IN_WIDTHS = (256, 256, 512, 512, 512, 512, 1024, 256, 256, 256, 256, 256, 256, 48)
OFFS = [0] + [int(o) for o in np.cumsum(IN_WIDTHS)]
ALPHA = (2 * DEPTH) ** 0.25
LN_EPS = 1e-5
NEG = -1e9
NCORES = 8
DBG = {}


class Buf:
    __slots__ = ("t", "w", "r", "name", "excl")

    def __init__(self, t, name="", excl=False):
        self.excl = excl
        self.t = t
        self.w = None
        self.r = {}
        self.name = name

    def __getitem__(self, k):
        return self.t[k]


class Prog:
    ENG = ("pe", "dve", "act", "pool", "sp")

    def __init__(self, nc, es, nds=20):
        self.nc = nc
        self.es = es
        self.eng = {"pe": nc.tensor, "dve": nc.vector, "act": nc.scalar, "pool": nc.gpsimd, "sp": nc.sync}
        self.sems = []
        self.sem = {}
        for e in self.ENG:
            self.sem[e] = len(self.sems)
            self.sems.append(es.enter_context(nc.semaphore("es_" + e)))
        self.cnt = {e: 0 for e in self.ENG}
        self.seen = {e: {} for e in self.ENG}
        self.dsem = []
        for i in range(nds):
            self.dsem.append(len(self.sems))
            self.sems.append(es.enter_context(nc.semaphore("ds%d" % i)))
        self.dval = [0] * nds
        self.dnext = 0
        self.stack = []
        self.uid = 0

    def push(self):
        st = ExitStack()
        self.stack.append(st)
        return st

    def pop(self):
        self.barrier()
        self.stack.pop().close()

    def _ctx(self):
        return self.stack[-1] if self.stack else self.es

    def sb(self, name, shape, dt):
        self.uid += 1
        return Buf(self._ctx().enter_context(self.nc.sbuf_tensor("%s_%d" % (name, self.uid), list(shape), dt)), name)

    def ps(self, name, shape, dt):
        self.uid += 1
        return Buf(self._ctx().enter_context(self.nc.psum_tensor("%s_%d" % (name, self.uid), list(shape), dt)), name, excl=True)

    def dram(self, name, shape, dt, kind):
        return Buf(self.nc.dram_tensor(name, list(shape), dt, kind=kind).ap(), name)

    def _wt(self, e, tok):
        if tok is None:
            return
        si, val, src = tok
        if src == e and e == "pe":
            return
        if self.seen[e].get(si, 0) >= val:
            return
        self.eng[e].wait_ge(self.sems[si], val)
        self.seen[e][si] = val

    def _deps(self, e, r, w):
        for b in r:
            self._wt(e, b.w)
            if b.excl:
                for k, t in b.r.items():
                    if k != e:
                        self._wt(e, t)
        for b in w:
            self._wt(e, b.w)
            for t in b.r.values():
                self._wt(e, t)

    def op(self, e, fn, r=(), w=()):
        self._deps(e, r, w)
        inst = fn(self.eng[e])
        self.cnt[e] += 1
        inst.then_inc(self.sems[self.sem[e]], 1)
        tok = (self.sem[e], self.cnt[e], e)
        for b in w:
            b.w = tok
            b.r = {}
        for b in r:
            b.r[e] = tok
        return tok

    def dma(self, q, out, in_, r=(), w=(), **kw):
        self._deps(q, r, w)
        i = self.dnext
        self.dnext = (i + 1) % len(self.dsem)
        if self.dval[i]:
            self._wt(q, (self.dsem[i], self.dval[i], "dma"))
        inst = self.eng[q].dma_start(out=out, in_=in_, **kw)
        self.dval[i] += 16
        inst.then_inc(self.sems[self.dsem[i]], 16)
        tok = (self.dsem[i], self.dval[i], "dma")
        for b in w:
            b.w = tok
            b.r = {}
        for b in r:
            b.r["d%d" % i] = tok
        return tok

    def idma(self, out, in_, off_ap, r=(), w=()):
        q = "pool"
        self._deps(q, r, w)
        i = self.dnext
        self.dnext = (i + 1) % len(self.dsem)
        if self.dval[i]:
            self._wt(q, (self.dsem[i], self.dval[i], "dma"))
        inst = self.nc.gpsimd.indirect_dma_start(
            out=out, out_offset=None, in_=in_, in_offset=bass.IndirectOffsetOnAxis(ap=off_ap, axis=0))
        self.dval[i] += 16
        inst.then_inc(self.sems[self.dsem[i]], 16)
        tok = (self.dsem[i], self.dval[i], "dma")
        for b in w:
            b.w = tok
            b.r = {}
        for b in r:
            b.r["d%d" % i] = tok
        return tok

    def barrier(self):
        for e in self.ENG:
            for e2 in self.ENG:
                if self.cnt[e2] and not (e == e2 == "pe"):
                    self._wt(e, (self.sem[e2], self.cnt[e2], e2))
            for i in range(len(self.dsem)):
                if self.dval[i]:
                    self._wt(e, (self.dsem[i], self.dval[i], "dma"))

    def finish(self):
        self.barrier()


def tt(P, e, out, a, b, op, r, w):
    return P.op(e, lambda g: g.tensor_tensor(out=out, in0=a, in1=b, op=op), r=r, w=w)


def ts(P, e, out, a, s1, s2, op0, op1, r, w):
    if s2 is None:
        return P.op(e, lambda g: g.tensor_scalar(out=out, in0=a, scalar1=s1, scalar2=None, op0=op0), r=r, w=w)
    return P.op(e, lambda g: g.tensor_scalar(out=out, in0=a, scalar1=s1, scalar2=s2, op0=op0, op1=op1), r=r, w=w)


def act(P, out, a, func, r, w, bias=None, scale=None, accum=None):
    kw = {}
    if bias is not None:
        kw["bias"] = bias
    if scale is not None:
        kw["scale"] = scale
    if accum is not None:
        kw["accum_out"] = accum
    return P.op("act", lambda g: g.activation(out=out, in_=a, func=func, **kw), r=r, w=w)


def mm(P, out, lhsT, rhs, start, stop, r, w):
    return P.op("pe", lambda g: g.matmul(out, lhsT=lhsT, rhs=rhs, start=start, stop=stop), r=r, w=w)


def tr(P, out, in_, ident, r, w):
    return P.op("pe", lambda g: g.transpose(out, in_, ident), r=r, w=w)


class Ctx:
    pass


def mean_rstd(P, tmp, src_ap, src_bufs, eps_buf):
    st6, mv, sd, rstd = tmp
    P.op("dve", lambda g: g.bn_stats(out=st6[:, :], in_=src_ap), r=src_bufs, w=[st6])
    P.op("dve", lambda g: g.bn_aggr(out=mv[:, :], in_=st6[:, :]), r=[st6], w=[mv])
    act(P, sd[:, :], mv[:, 1:2], AF.Sqrt, r=[mv, eps_buf], w=[sd], bias=eps_buf[:, 0:1])
    P.op("dve", lambda g: g.reciprocal(out=rstd[:, :], in_=sd[:, :]), r=[sd], w=[rstd])
    return mv, rstd


def stat_tmp(P, tag):
    return (P.sb("st6" + tag, [128, 6], F32), P.sb("mv" + tag, [128, 2], F32),
            P.sb("sd" + tag, [128, 1], F32), P.sb("rstd" + tag, [128, 1], F32))


WFR, WTR, WTS, WFN, WTN = 512, 512, 512, 1024, 280
C_WFR = 0
C_WTR = C_WFR + WFR
C_WTS = C_WTR + WTR
C_WFN = C_WTS + WTS
C_WTN = C_WFN + WFN
WA_COLS = C_WTN + WTN
T = SEQ
NT = T // 128


def build_stage_a(phases="xrsn"):
    nc = bass.Bass("TRN2", target_bir_lowering=False)
    es = ExitStack()
    with es:
        P = Prog(nc, es)
        C = Ctx()
        din = lambda n, s, dt=F32: P.dram(n, s, dt, "ExternalInput")
        C.x = din("x", [T, D_MODEL])
        C.w = din("w", [D_MODEL, WA_COLS])
        C.cosT = din("cosT", [64, T])
        C.sinT = din("sinT", [64, T])
        C.dinT = din("dinT", [128, 2, 128])
        C.xiT = din("xiT", [64, 2, 128])
        C.zeta = din("zeta", [128, 2])
        C.gch = din("gch", [64, 2])
        C.gng = din("gng", [128, 256])
        C.gnb = din("gnb", [128, 256])
        C.lng = din("lng", [128, 256])
        C.lnb = din("lnb", [128, 256])
        C.sguw = din("sguw", [2, 128, 128])
        C.sgub = din("sgub", [128, 2])
        C.tril = din("tril", [128, 128])
        C.ident = din("ident", [128, 128])
        C.peT = din("peT", [64, 2, 32])
        C.w1 = din("w1", [2, 2048, 256])
        C.w2 = din("w2", [2, 256, 64])
        C.ovl = din("ovl", [127, 32])
        C.keep = din("keep", [128, NT, 32])
        C.addc = din("addc", [128, NT, 32])
        C.caus = din("caus", [128, 128])
        C.wmask = din("wmask", [128, 640])
        C.valid31 = din("valid31", [128, 1])
        C.cmask = din("cmask", [128, 247])
        C.y = P.dram("y", [T, 1024], F32, "ExternalOutput")

        C.eps = P.sb("eps", [128, 1], F32)
        P.op("dve", lambda g: g.memset(C.eps[:, :], LN_EPS), w=[C.eps])
        C.identb = P.sb("identb", [128, 128], BF16)
        P.dma("pool", C.identb[:, :], C.ident[:, :], w=[C.identb])
        C.xT = [P.sb("xT%d" % g, [128, 16, 512], BF16) for g in range(4)]
        if "x" in phases:
            load_xT(P, C)
        if "r" in phases:
            phase_r(P, C)
        if "s" in phases:
            phase_s(P, C)
        P.push()
        C.nqT = [P.sb("nqT%d" % h, [64, T], BF16) for h in range(8)]
        C.kcT = [P.sb("kcT%d" % g, [64, T], BF16) for g in range(2)]
        C.vcT = [P.sb("vcT%d" % g, [64, T], BF16) for g in range(2)]
        C.kslT = [P.sb("kslT%d" % g, [64, T], BF16) for g in range(2)]
        C.kwT = [P.sb("kwT%d" % g, [64, T], BF16) for g in range(2)]
        C.vsl = P.sb("vsl", [128, NT, 128], BF16)
        C.vw = P.sb("vw", [128, NT, 128], BF16)
        C.gates = P.sb("gates", [128, NT, 24], F32)
        if "n" in phases:
            phase_n_proj(P, C)
            phase_n_attn(P, C)
        P.pop()
        P.finish()
    return nc


def load_xT(P, C):
    P.push()
    xb = [P.sb("xb%d" % i, [128, D_MODEL], BF16) for i in range(2)]
    tp = [P.ps("tp%d" % i, [128, 1024], BF16) for i in range(2)]
    n = 0
    for i in range(NT):
        b = xb[i % 2]
        P.dma("pool", b[:, :], C.x[i * 128:(i + 1) * 128, :], w=[b])
        g, off = i // 4, (i % 4) * 128
        for half in range(2):
            t = tp[n % 2]
            for kk in range(8):
                k = half * 8 + kk
                tr(P, t[:, kk * 128:(kk + 1) * 128], b[:, k * 128:(k + 1) * 128], C.identb[:, :], r=[b, C.identb], w=[t])
            src = t[:, :].rearrange("p (k c) -> p k c", k=8)
            dst = C.xT[g][:, half * 8:half * 8 + 8, off:off + 128]
            if n % 2 == 0:
                P.op("dve", lambda e: e.tensor_copy(out=dst, in_=src), r=[t], w=[C.xT[g]])
            else:
                P.op("act", lambda e: e.copy(out=dst, in_=src), r=[t], w=[C.xT[g]])
            n += 1
    P.pop()


def load_w(P, C, buf, c0, ncols):
    P.dma("pool", buf[:, :, 0:ncols], C.w[:, c0:c0 + ncols].rearrange("(k p) c -> p k c", p=128), w=[buf])


def proj_fm(P, C, ps, wbuf, c0, tg, m=64):
    for k in range(16):
        mm(P, ps[0:m, :], wbuf[:, k, c0:c0 + m], C.xT[tg][:, k, :], k == 0, k == 15, r=[wbuf, C.xT[tg]], w=[ps])


def proj_tm(P, C, ps, wbuf, c0, ncols, i):
    g, off = i // 4, (i % 4) * 128
    for k in range(16):
        mm(P, ps[:, 0:ncols], C.xT[g][:, k, off:off + 128], wbuf[:, k, c0:c0 + ncols], k == 0, k == 15,
           r=[wbuf, C.xT[g]], w=[ps])


def phase_r(P, C):
    P.push()
    wtr = P.sb("wtr", [128, 16, 512], BF16)
    wfr = wtr
    load_w(P, C, wtr, C_WTR, 512)
    small = {}
    for nm, shp in (("dinT", [128, 2, 128]), ("xiT", [64, 2, 128]), ("zeta", [128, 2]), ("gch", [64, 2]),
                    ("gng", [128, 256]), ("gnb", [128, 256])):
        small[nm] = P.sb(nm, shp, F32)
        src = getattr(C, nm)
        P.dma("sp", small[nm].t[tuple(slice(None) for _ in shp)], src.t[tuple(slice(None) for _ in shp)], w=[small[nm]])
    qT = [P.sb("rqT%d" % h, [64, T], BF16) for h in range(2)]
    kT = [P.sb("rkT%d" % h, [64, T], BF16) for h in range(2)]
    v_sb = P.sb("rv", [128, NT, 256], BF16)
    sg = P.sb("rsg", [128, NT, 256], F32)

    P.push()
    pf = [P.ps("pf%d" % i, [128, 512], F32) for i in range(4)]
    pt = [P.ps("pt%d" % i, [128, 512], F32) for i in range(2)]
    t1 = [P.sb("t1_%d" % i, [64, 512], F32) for i in range(2)]
    t2 = [P.sb("t2_%d" % i, [64, 512], F32) for i in range(2)]
    cosT = P.sb("cosT", [64, T], F32)
    sinT = P.sb("sinT", [64, T], F32)
    P.dma("sp", cosT[:, :], C.cosT[:, :], w=[cosT])
    P.dma("sp", sinT[:, :], C.sinT[:, :], w=[sinT])
    for i in range(0 if DBG.get("r_notm") else NT):
        p = pt[i % 2]
        proj_tm(P, C, p, wtr, 0, 512, i)
        if not DBG.get("r_nocopy"):
            P.op("dve", lambda e: e.tensor_copy(out=v_sb[:, i, :], in_=p[:, 0:256]), r=[p], w=[v_sb])
        if DBG.get("r_nosilu"):
            P.op("dve", lambda e: e.tensor_copy(out=sg[:, i, :], in_=p[:, 256:512]), r=[p], w=[sg])
        else:
            act(P, sg[:, i, :], p[:, 256:512], AF.Silu, r=[p] + ([v_sb] if DBG.get('r_ser') else []), w=[sg])
    load_w(P, C, wfr, C_WFR, 512)
    n = 0
    for hl in range(0 if DBG.get("r_nofm") else 2):
        for tg in range(4):
            sl = slice(tg * 512, (tg + 1) * 512)
            for j, dst in ((0, qT[hl]), (2, kT[hl])):
                pa, pb = pf[(n % 2) * 2], pf[(n % 2) * 2 + 1]
                a, b = t1[n % 2], t2[n % 2]
                proj_fm(P, C, pa, wfr, hl * 256 + j * 64, tg)
                proj_fm(P, C, pb, wfr, hl * 256 + (j + 1) * 64, tg)
                tt(P, "dve", a[:, :], pa[0:64, :], cosT[:, sl], ALU.mult, r=[pa, cosT], w=[a])
                tt(P, "dve", b[:, :], pb[0:64, :], sinT[:, sl], ALU.mult, r=[pb, sinT], w=[b])
                tt(P, "pool", dst[:, sl], a[:, :], b[:, :], ALU.add, r=[a, b], w=[dst])
                n += 1
    P.pop()

    if DBG.get("r_noloop"):
        P.pop()
        return
    P.push()
    att_ps = [P.ps("att%d" % h, [128, 512], F32) for h in range(2)]
    y_ps = [P.ps("yps%d" % h, [128, 512], F32) for h in range(2)]
    ktr_ps = [P.ps("ktr%d" % h, [128, 1024], BF16) for h in range(2)]
    st_ps = [P.ps("stp%d" % h, [128, 512], F32) for h in range(2)]
    att_sb = [P.sb("attsb%d" % h, [128, 128], BF16) for h in range(2)]
    qx = [P.sb("qx%d" % h, [64, 128], BF16) for h in range(2)]
    kz = [P.sb("kz%d" % h, [128, 64], BF16) for h in range(2)]
    state = [P.sb("state%d" % h, [64, 128], F32) for h in range(2)]
    state_bf = [P.sb("statebf%d" % h, [64, 128], BF16) for h in range(2)]
    yn = [P.sb("yn%d" % h, [128, 128], F32) for h in range(2)]
    stt = [stat_tmp(P, "r%d" % h) for h in range(2)]
    yout = [P.sb("yout%d" % i, [128, 256], F32) for i in range(2)]
    dinT, xiT, zeta, gch, gng, gnb = (small[k] for k in ("dinT", "xiT", "zeta", "gch", "gng", "gnb"))
    for hl in range(2):
        P.op("dve", lambda e: e.memset(state[hl][:, :], 0.0), w=[state[hl]])
    for i in range(NT):
        cs = slice(i * 128, (i + 1) * 128)
        yo = yout[i % 2]
        for hl in range(2):
            hs = slice(hl * 128, (hl + 1) * 128)
            mm(P, att_ps[hl][:, 0:128], kT[hl][:, cs], qT[hl][:, cs], True, True, r=[kT[hl], qT[hl]], w=[att_ps[hl]])
            tt(P, "dve", att_sb[hl][:, :], att_ps[hl][:, 0:128], dinT[:, hl, :], ALU.mult, r=[att_ps[hl], dinT], w=[att_sb[hl]])
            mm(P, y_ps[hl][:, 0:128], att_sb[hl][:, :], v_sb[:, i, hs], True, i == 0, r=[att_sb[hl], v_sb], w=[y_ps[hl]])
            if i > 0:
                tt(P, "pool", qx[hl][:, :], qT[hl][:, cs], xiT[:, hl, :], ALU.mult, r=[qT[hl], xiT], w=[qx[hl]])
                mm(P, y_ps[hl][:, 0:128], qx[hl][:, :], state_bf[hl][:, :], False, True, r=[qx[hl], state_bf[hl]], w=[y_ps[hl]])
            if i < NT - 1:
                tr(P, ktr_ps[hl][:, 0:64], kT[hl][:, cs], C.identb[0:64, 0:64], r=[kT[hl], C.identb], w=[ktr_ps[hl]])
                ts(P, "dve", kz[hl][:, :], ktr_ps[hl][:, 0:64], zeta[:, hl:hl + 1], None, ALU.mult, None, r=[ktr_ps[hl], zeta], w=[kz[hl]])
                mm(P, st_ps[hl][0:64, 0:128], kz[hl][:, :], v_sb[:, i, hs], True, True, r=[kz[hl], v_sb], w=[st_ps[hl]])
                P.op("dve", lambda e: e.scalar_tensor_tensor(out=state[hl][:, :], in0=state[hl][:, :], scalar=gch[:, hl:hl + 1],
                                                             in1=st_ps[hl][0:64, 0:128], op0=ALU.mult, op1=ALU.add),
                     r=[state[hl], st_ps[hl], gch], w=[state[hl]])
                P.op("act", lambda e: e.copy(out=state_bf[hl][:, :], in_=state[hl][:, :]), r=[state[hl]], w=[state_bf[hl]])
            mv, rstd = mean_rstd(P, stt[hl], y_ps[hl][:, 0:128], [y_ps[hl]], C.eps)
            ts(P, "dve", yn[hl][:, :], y_ps[hl][:, 0:128], mv[:, 0:1], rstd[:, 0:1], ALU.subtract, ALU.mult,
               r=[y_ps[hl], mv, rstd], w=[yn[hl]])
            tt(P, "pool", yn[hl][:, :], yn[hl][:, :], gng[:, hs], ALU.mult, r=[yn[hl], gng], w=[yn[hl]])
            tt(P, "pool", yn[hl][:, :], yn[hl][:, :], gnb[:, hs], ALU.add, r=[yn[hl], gnb], w=[yn[hl]])
            tt(P, "pool", yo[:, hs], yn[hl][:, :], sg[:, i, hs], ALU.mult, r=[yn[hl], sg], w=[yo])
        P.dma("sp", C.y[cs, 0:256], yo[:, :], r=[yo])
    P.pop()
    P.pop()


def phase_s(P, C):
    P.push()
    wts = P.sb("wts", [128, 16, 512], BF16)
    load_w(P, C, wts, C_WTS, 512)
    lng = P.sb("lng", [128, 256], F32)
    lnb = P.sb("lnb", [128, 256], F32)
    sgub = P.sb("sgub", [128, 2], F32)
    tril = P.sb("tril", [128, 128], F32)
    P.dma("sp", lng[:, :], C.lng[:, :], w=[lng])
    P.dma("sp", lnb[:, :], C.lnb[:, :], w=[lnb])
    P.dma("sp", sgub[:, :], C.sgub[:, :], w=[sgub])
    P.dma("sp", tril[:, :], C.tril[:, :], w=[tril])
    wraw = P.sb("wraw", [128, 2, 128], F32)
    wmb = P.sb("wmb", [128, 2, 128], BF16)
    wTm = P.sb("wTm", [128, 2, 128], BF16)
    pt = [P.ps("spt%d" % i, [128, 512], F32) for i in range(2)]
    s_ps = [P.ps("sps%d" % g, [128, 512], F32) for g in range(2)]
    wtp = P.ps("wtp", [128, 1024], BF16)
    for gl in range(2):
        P.dma("sp", wraw[:, gl, :], C.sguw[gl, :, :], w=[wraw])
    for gl in range(2):
        tt(P, "dve", wmb[:, gl, :], wraw[:, gl, :], tril[:, :], ALU.mult, r=[wraw, tril], w=[wmb])
        tr(P, wtp[:, 0:128], wmb[:, gl, :], C.identb[:, :], r=[wmb, C.identb], w=[wtp])
        P.op("dve", lambda e: e.tensor_copy(out=wTm[:, gl, :], in_=wtp[:, 0:128]), r=[wtp], w=[wTm])
    u = [P.sb("su%d" % i, [128, 256], F32) for i in range(2)]
    v = [P.sb("sv%d" % i, [128, 256], F32) for i in range(2)]
    vn = [P.sb("svn%d" % g, [128, 128], F32) for g in range(2)]
    vnb = [P.sb("svnb%d" % g, [128, 128], BF16) for g in range(2)]
    stt = [stat_tmp(P, "s%d" % g) for g in range(2)]
    yout = [P.sb("syout%d" % i, [128, 256], F32) for i in range(2)]
    for i in range(NT):
        cs = slice(i * 128, (i + 1) * 128)
        p, ui, vi, yo = pt[i % 2], u[i % 2], v[i % 2], yout[i % 2]
        proj_tm(P, C, p, wts, 0, 512, i)
        act(P, ui[:, :], p[:, 0:256], AF.Gelu, r=[p], w=[ui])
        act(P, vi[:, :], p[:, 256:512], AF.Gelu, r=[p], w=[vi])
        for gl in range(2):
            gs = slice(gl * 128, (gl + 1) * 128)
            mv, rstd = mean_rstd(P, stt[gl], vi[:, gs], [vi], C.eps)
            ts(P, "dve", vn[gl][:, :], vi[:, gs], mv[:, 0:1], rstd[:, 0:1], ALU.subtract, ALU.mult, r=[vi, mv, rstd], w=[vn[gl]])
            tt(P, "pool", vn[gl][:, :], vn[gl][:, :], lng[:, gs], ALU.mult, r=[vn[gl], lng], w=[vn[gl]])
            tt(P, "pool", vnb[gl][:, :], vn[gl][:, :], lnb[:, gs], ALU.add, r=[vn[gl], lnb], w=[vnb[gl]])
            mm(P, s_ps[gl][:, 0:128], wTm[:, gl, :], vnb[gl][:, :], True, True, r=[wTm, vnb[gl]], w=[s_ps[gl]])
            P.op("dve", lambda e: e.scalar_tensor_tensor(out=yo[:, gs], in0=s_ps[gl][:, 0:128], scalar=sgub[:, gl:gl + 1],
                                                         in1=ui[:, gs], op0=ALU.add, op1=ALU.mult),
                 r=[s_ps[gl], sgub, ui], w=[yo])
        P.dma("sp", C.y[cs, 256:512], yo[:, :], r=[yo])
    P.pop()


def phase_n_proj(P, C):
    P.push()
    wfn1 = P.sb("wfn", [128, 16, 512], BF16)
    wfn = [wfn1, wfn1]
    wtn = P.sb("wtn", [128, 16, WTN], BF16)
    load_w(P, C, wfn1, C_WFN, 512)
    load_w(P, C, wtn, C_WTN, WTN)
    pf = [P.ps("npf%d" % i, [128, 512], F32) for i in range(4)]
    pt = [P.ps("npt%d" % i, [128, 512], F32) for i in range(2)]
    dsts = C.nqT + C.kcT + C.vcT + C.kslT + C.kwT
    n = 0
    for pc in range(16):
        if pc == 8:
            load_w(P, C, wfn1, C_WFN + 512, 512)
        wb, c0 = wfn[pc // 8], (pc % 8) * 64
        scale = 0.125 if pc < 8 else 1.0
        for tg in range(4):
            p = pf[n % 4]
            proj_fm(P, C, p, wb, c0, tg)
            sl = slice(tg * 512, (tg + 1) * 512)
            dst = dsts[pc]
            if n % 2 == 0:
                ts(P, "dve", dst[:, sl], p[0:64, :], scale, None, ALU.mult, None, r=[p], w=[dst])
            else:
                act(P, dst[:, sl], p[0:64, :], AF.Copy, r=[p], w=[dst], scale=scale)
            n += 1
    for i in range(NT):
        p = pt[i % 2]
        proj_tm(P, C, p, wtn, 0, WTN, i)
        P.op("dve", lambda e: e.tensor_copy(out=C.vsl[:, i, :], in_=p[:, 0:128]), r=[p], w=[C.vsl])
        P.op("dve", lambda e: e.tensor_copy(out=C.vw[:, i, :], in_=p[:, 128:256]), r=[p], w=[C.vw])
        act(P, C.gates[:, i, :], p[:, 256:280], AF.Sigmoid, r=[p], w=[C.gates])
    P.pop()


def phase_n_attn(P, C):
    P.push()
    kcmpT = [P.sb("kcmpT%d" % g, [64, 128], BF16) for g in range(2)]
    vcmp = [P.sb("vcmp%d" % g, [128, 64], BF16) for g in range(2)]
    P.push()
    w1 = [P.sb("cw1_%d" % j, [64, 32, 256], BF16) for j in range(2)]
    w2 = [P.sb("cw2_%d" % j, [128, 2, 64], BF16) for j in range(2)]
    peT = P.sb("peT", [64, 2, 32], BF16)
    P.dma("pool", peT[:, :, :], C.peT[:, :, :], w=[peT])
    for j in range(2):
        for ph in range(4):
            P.dma("pool", w1[j][:, ph * 8:(ph + 1) * 8, :],
                  C.w1[j, ph * 512:(ph + 1) * 512, :].rearrange("(p d) h -> d p h", d=64), w=[w1[j]])
        P.dma("pool", w2[j][:, :, :], C.w2[j, :, :].rearrange("(c p) d -> p c d", p=128), w=[w2[j]])
    hp = [P.ps("hp%d" % i, [128, 512], F32) for i in range(2)]
    bp = P.ps("bp", [128, 512], F32)
    op_ = P.ps("cop", [128, 512], F32)
    pebias = P.sb("pebias", [128, 2], F32)
    hid = [P.sb("hid%d" % c, [128, 128], BF16) for c in range(2)]
    n = 0
    for j in range(2):
        srcs = C.kcT if j == 0 else C.vcT
        for c in range(2):
            for p_ in range(32):
                mm(P, bp[:, c:c + 1], w1[j][:, p_, c * 128:(c + 1) * 128], peT[:, j, p_:p_ + 1], p_ == 0, p_ == 31,
                   r=[w1[j], peT], w=[bp])
        P.op("dve", lambda e: e.tensor_copy(out=pebias[:, :], in_=bp[:, 0:2]), r=[bp], w=[pebias])
        for gl in range(2):
            for c in range(2):
                h_ = hp[n % 2]
                n += 1
                for p_ in range(32):
                    mm(P, h_[:, 0:127], w1[j][:, p_, c * 128:(c + 1) * 128], srcs[gl][:, p_:p_ + 16 * 126 + 1:16],
                       p_ == 0, p_ == 31, r=[w1[j], srcs[gl]], w=[h_])
                act(P, hid[c][:, 0:127], h_[:, 0:127], AF.Gelu, r=[h_, pebias], w=[hid[c]], bias=pebias[:, c:c + 1])
            if j == 0:
                for c in range(2):
                    mm(P, op_[0:64, 0:127], w2[j][:, c, :], hid[c][:, 0:127], c == 0, c == 1, r=[w2[j], hid[c]], w=[op_])
                P.op("dve", lambda e: e.tensor_copy(out=kcmpT[gl][:, 0:127], in_=op_[0:64, 0:127]), r=[op_], w=[kcmpT[gl]])
            else:
                for c in range(2):
                    mm(P, op_[0:127, 0:64], hid[c][:, 0:127], w2[j][:, c, :], c == 0, c == 1, r=[w2[j], hid[c]], w=[op_])
                P.op("dve", lambda e: e.tensor_copy(out=vcmp[gl][0:127, :], in_=op_[0:127, 0:64]), r=[op_], w=[vcmp[gl]])
    P.pop()

    ovl = P.sb("ovl", [128, 32], BF16)
    P.dma("pool", ovl[0:127, :], C.ovl[:, :], w=[ovl])
    keep = P.sb("keep", [128, NT, 32], F32)
    addc = P.sb("addc", [128, NT, 32], F32)
    caus = P.sb("caus", [128, 128], F32)
    wmask = P.sb("wmask", [128, 640], F32)
    valid31 = P.sb("valid31", [128, 1], F32)
    cmask = P.sb("cmask", [128, 247], F32)
    P.dma("sp", cmask[:, :], C.cmask[:, :], w=[cmask])
    P.dma("sp", keep[:, :, :], C.keep[:, :, :], w=[keep])
    P.dma("sp", addc[:, :, :], C.addc[:, :, :], w=[addc])
    P.dma("sp", caus[:, :], C.caus[:, :], w=[caus])
    P.dma("sp", wmask[:, :], C.wmask[:, :], w=[wmask])
    P.dma("sp", valid31[:, :], C.valid31[:, :], w=[valid31])

    s_ps = [P.ps("nsps%d" % i, [128, 512], F32) for i in range(4)]
    eT_ps = [P.ps("neT%d" % i, [128, 1024], BF16) for i in range(2)]
    o_ps = P.ps("nops", [128, 512], F32)
    imp_ps = P.ps("nimp", [128, 512], F32)

    sm = P.sb("nsm", [128, T], F32)
    e_bf = P.sb("ne_bf", [128, T], BF16)
    eT_sb = P.sb("neT_sb", [128, NT, 128], BF16)
    pc_f = P.sb("npc_f", [128, 128], F32)
    pc_bf = P.sb("npc_bf", [128, 128], BF16)
    P.op("dve", lambda e: e.memset(pc_bf[:, :], 0.0), w=[pc_bf])
    negm = P.sb("nnegm", [128, 1], F32)
    rsum = P.sb("nrsum", [128, 1], F32)
    rinv = P.sb("nrinv", [128, 1], F32)
    gr = P.sb("ngr", [128, 1], F32)
    imp2 = P.sb("nimp2", [128, 32], F32)
    top8 = P.sb("ntop8", [128, 8], F32)
    selb = P.sb("nselb", [128, 32], F32)
    maskf = P.sb("nmaskf", [128, T], F32)
    oacc = [P.sb("noacc%d" % i, [128, 512], F32) for i in range(2)]

    def softmax_rows(nk, mask_ap, mask_bufs):
        for cb in range((nk + 511) // 512):
            w_ = min(512, nk - cb * 512)
            tt(P, "dve", sm[:, cb * 512:cb * 512 + w_], s_ps[cb][:, 0:w_], mask_ap[:, cb * 512:cb * 512 + w_], ALU.add,
               r=[s_ps[cb]] + mask_bufs, w=[sm])
        P.op("dve", lambda e: e.tensor_reduce(out=negm[:, :], in_=sm[:, 0:nk], axis=AX.X, op=ALU.max, negate=True),
             r=[sm], w=[negm])
        act(P, e_bf[:, 0:nk], sm[:, 0:nk], AF.Exp, r=[sm, negm], w=[e_bf, rsum], bias=negm[:, 0:1], accum=rsum[:, 0:1])
        P.op("dve", lambda e: e.reciprocal(out=rinv[:, :], in_=rsum[:, :]), r=[rsum], w=[rinv])

    def pv(nblk, vbuf, vcol, vb0, ocol):
        for kb in range(nblk):
            t = eT_ps[kb // 8]
            tr(P, t[:, (kb % 8) * 128:(kb % 8 + 1) * 128], e_bf[:, kb * 128:(kb + 1) * 128], C.identb[:, :],
               r=[e_bf, C.identb], w=[t])
        for hb in range((nblk + 7) // 8):
            nb = min(8, nblk - hb * 8)
            src = eT_ps[hb][:, 0:nb * 128].rearrange("p (k c) -> p k c", k=nb)
            dst = eT_sb[:, hb * 8:hb * 8 + nb, :]
            if hb == 0:
                P.op("act", lambda e: e.copy(out=dst, in_=src), r=[eT_ps[hb]], w=[eT_sb])
            else:
                P.op("dve", lambda e: e.tensor_copy(out=dst, in_=src), r=[eT_ps[hb]], w=[eT_sb])
        for kb in range(nblk):
            mm(P, o_ps[:, ocol], eT_sb[:, kb, :], vbuf[:, vb0 + kb, vcol], kb == 0, kb == nblk - 1, r=[eT_sb, vbuf], w=[o_ps])

    for i in range(NT):
        cs = slice(i * 128, (i + 1) * 128)
        oa = oacc[i % 2]
        for gl in range(2):
            vcol = slice(gl * 64, (gl + 1) * 64)
            for hq in range(4):
                h = gl * 4 + hq
                hs = slice(h * 64, (h + 1) * 64)
                mm(P, s_ps[0][:, 0:127], C.nqT[h][:, cs], kcmpT[gl][:, 0:127], True, True, r=[C.nqT[h], kcmpT[gl]], w=[s_ps[0]])
                tt(P, "dve", sm[:, 0:127], s_ps[0][:, 0:127], cmask[:, 120 - 8 * i:247 - 8 * i], ALU.add,
                   r=[s_ps[0], cmask], w=[sm])
                P.op("dve", lambda e: e.tensor_reduce(out=negm[:, :], in_=sm[:, 0:127], axis=AX.X, op=ALU.max, negate=True),
                     r=[sm], w=[negm])
                act(P, pc_f[:, 0:127], sm[:, 0:127], AF.Exp, r=[sm, negm], w=[pc_f, rsum], bias=negm[:, 0:1], accum=rsum[:, 0:1])
                P.op("dve", lambda e: e.reciprocal(out=rinv[:, :], in_=rsum[:, :]), r=[rsum], w=[rinv])
                if i == 0:
                    tt(P, "dve", rinv[:, :], rinv[:, :], valid31[:, :], ALU.mult, r=[rinv, valid31], w=[rinv])
                ts(P, "dve", pc_bf[:, 0:127], pc_f[:, 0:127], rinv[:, 0:1], None, ALU.mult, None, r=[pc_f, rinv], w=[pc_bf])
                tr(P, eT_ps[0][:, 0:128], pc_bf[:, :], C.identb[:, :], r=[pc_bf, C.identb], w=[eT_ps[0]])
                P.op("act", lambda e: e.copy(out=eT_sb[:, 0, :], in_=eT_ps[0][:, 0:128]), r=[eT_ps[0]], w=[eT_sb])
                mm(P, o_ps[:, 0:64], eT_sb[0:127, 0, :], vcmp[gl][0:127, :], True, True, r=[eT_sb, vcmp[gl]], w=[o_ps])
                mm(P, imp_ps[:, 0:32], eT_sb[0:127, 0, :], ovl[0:127, :], hq == 0, hq == 3, r=[eT_sb, ovl], w=[imp_ps])
                ts(P, "dve", oa[:, hs], o_ps[:, 0:64], C.gates[:, i, 3 * h:3 * h + 1], None, ALU.mult, None,
                   r=[o_ps, C.gates], w=[oa])
            tt(P, "dve", imp2[:, :], imp_ps[:, 0:32], keep[:, i, :], ALU.mult, r=[imp_ps, keep], w=[imp2])
            tt(P, "dve", imp2[:, :], imp2[:, :], addc[:, i, :], ALU.add, r=[imp2, addc], w=[imp2])
            P.op("dve", lambda e: e.max(out=top8[:, :], in_=imp2[:, :]), r=[imp2], w=[top8])
            ts(P, "dve", selb[:, :], imp2[:, :], top8[:, 7:8], NEG, ALU.is_lt, ALU.mult, r=[imp2, top8], w=[selb])
            nb64 = 2 * (i + 1)
            nk = 128 * (i + 1)
            P.op("dve", lambda e: e.tensor_copy(out=maskf[:, 0:nk].rearrange("p (j l) -> p j l", l=64),
                                                in_=selb[:, 0:nb64].unsqueeze(2).to_broadcast([128, nb64, 64])),
                 r=[selb], w=[maskf])
            tt(P, "dve", maskf[:, i * 128:nk], maskf[:, i * 128:nk], caus[:, :], ALU.add, r=[maskf, caus], w=[maskf])
            for hq in range(4):
                h = gl * 4 + hq
                hs = slice(h * 64, (h + 1) * 64)
                for cb in range((nk + 511) // 512):
                    w_ = min(512, nk - cb * 512)
                    mm(P, s_ps[cb][:, 0:w_], C.nqT[h][:, cs], C.kslT[gl][:, cb * 512:cb * 512 + w_], True, True,
                       r=[C.nqT[h], C.kslT[gl]], w=[s_ps[cb]])
                softmax_rows(nk, maskf, [maskf])
                pv(i + 1, C.vsl, vcol, 0, slice(64, 128))
                tt(P, "dve", gr[:, :], rinv[:, :], C.gates[:, i, 3 * h + 1:3 * h + 2], ALU.mult, r=[rinv, C.gates], w=[gr])
                P.op("dve", lambda e: e.scalar_tensor_tensor(out=oa[:, hs], in0=o_ps[:, 64:128], scalar=gr[:, 0:1],
                                                             in1=oa[:, hs], op0=ALU.mult, op1=ALU.add),
                     r=[o_ps, gr, oa], w=[oa])
                kt0 = max(0, i - 4)
                nwb = i - kt0 + 1
                nkw = nwb * 128
                for cb in range((nkw + 511) // 512):
                    w_ = min(512, nkw - cb * 512)
                    mm(P, s_ps[cb][:, 0:w_], C.nqT[h][:, cs], C.kwT[gl][:, kt0 * 128 + cb * 512:kt0 * 128 + cb * 512 + w_],
                       True, True, r=[C.nqT[h], C.kwT[gl]], w=[s_ps[cb]])
                softmax_rows(nkw, wmask[:, 640 - nkw:640], [wmask])
                pv(nwb, C.vw, vcol, kt0, slice(128, 192))
                tt(P, "dve", gr[:, :], rinv[:, :], C.gates[:, i, 3 * h + 2:3 * h + 3], ALU.mult, r=[rinv, C.gates], w=[gr])
                P.op("dve", lambda e: e.scalar_tensor_tensor(out=oa[:, hs], in0=o_ps[:, 128:192], scalar=gr[:, 0:1],
                                                             in1=oa[:, hs], op0=ALU.mult, op1=ALU.add),
                     r=[o_ps, gr, oa], w=[oa])
        P.dma("sp", C.y[cs, 512:1024], oa[:, :], r=[oa])
    P.pop()


def _consts_a():
    c = {}
    half = 32
    inv = 10000.0 ** (-np.arange(half, dtype=np.float64) / half)
    ang = np.arange(T, dtype=np.float64)[None, :] * inv[:, None]
    cos = np.cos(ang)
    sin = np.sin(ang)
    c["cosT"] = np.concatenate([cos, cos], 0).astype(np.float32)
    c["sinT"] = np.concatenate([-sin, sin], 0).astype(np.float32)
    n = np.arange(128)
    c["tril"] = (n[:, None] >= n[None, :]).astype(np.float32)
    c["ident"] = np.eye(128, dtype=np.float32)
    cst = np.arange(127) * 16
    sst = np.arange(32) * 64
    c["ovl"] = ((cst[:, None] < sst[None, :] + 64) & (cst[:, None] + 32 > sst[None, :])).astype(np.float32)
    t = np.arange(T)
    jj = np.arange(32)[None, :]
    cur = (t // 64)[:, None]
    forced = (jj == 0) | (jj == cur) | (jj == cur - 1)
    causal = sst[None, :] <= t[:, None]
    keep = (causal & ~forced).astype(np.float32)
    addc = np.where(causal, np.where(forced, 1e6, 0.0), NEG).astype(np.float32)
    c["keep"] = np.ascontiguousarray(keep.reshape(NT, 128, 32).transpose(1, 0, 2))
    c["addc"] = np.ascontiguousarray(addc.reshape(NT, 128, 32).transpose(1, 0, 2))
    c["caus"] = np.where(n[None, :] <= n[:, None], 0.0, NEG).astype(np.float32)
    wm = np.zeros((128, 640), np.float32)
    wm[:, 0:128] = np.where(n[None, :] > n[:, None], 0.0, NEG)
    wm[:, 512:640] = c["caus"]
    c["wmask"] = wm
    c["valid31"] = (n >= 31).astype(np.float32)[:, None]
    xx = np.arange(247)
    c["cmask"] = np.where(16 * (xx[None, :] - 120) + 31 <= n[:, None], 0.0, NEG).astype(np.float32)
    return c


def _ret_tables(hh):
    heads = [2 * hh, 2 * hh + 1]
    gamma = 1.0 - 2.0 ** (-5.0 - np.arange(4, dtype=np.float64))
    lg = np.log(gamma)
    n = np.arange(128, dtype=np.float64)
    sc = 64 ** -0.5
    dinT = np.zeros((128, 2, 128), np.float32)
    xiT = np.zeros((64, 2, 128), np.float32)
    zeta = np.zeros((128, 2), np.float32)
    gch = np.zeros((64, 2), np.float32)
    for j, h in enumerate(heads):
        diff = n[None, :] - n[:, None]
        dinT[:, j, :] = np.where(diff >= 0, np.exp(lg[h] * np.maximum(diff, 0.0)), 0.0) * sc
        xiT[:, j, :] = np.exp(lg[h] * (n + 1.0))[None, :]
        zeta[:, j] = np.exp(lg[h] * (127.0 - n)) * sc
        gch[:, j] = np.exp(lg[h] * 128.0)
    return dinT, xiT, zeta, gch


def _wa_cols(hh):
    cols = []
    O = OFFS
    for hl in range(2):
        h = 2 * hh + hl
        for base in (O[0], O[1]):
            b = base + h * 64
            cols += list(range(b, b + 64))
            cols += list(range(b + 32, b + 64)) + list(range(b, b + 32))
    for base in (O[2], O[3]):
        cols += list(range(base + hh * 256, base + hh * 256 + 256))
    for base in (O[4], O[5]):
        cols += list(range(base + hh * 256, base + hh * 256 + 256))
    cols += list(range(O[6] + hh * 512, O[6] + hh * 512 + 512))
    for base in (O[7], O[8], O[9], O[11]):
        cols += list(range(base + hh * 128, base + hh * 128 + 128))
    for base in (O[10], O[12]):
        cols += list(range(base + hh * 128, base + hh * 128 + 128))
    cols += list(range(O[13] + hh * 24, O[13] + hh * 24 + 24))
    assert len(cols) == WA_COLS
    return np.asarray(cols)


def rep128(v):
    return np.ascontiguousarray(np.broadcast_to(np.asarray(v, np.float32)[None, :], (128, v.shape[0])))


def stage_a_inputs(x, inp, l, consts):
    maps = []
    per_half = {}
    for hh in range(2):
        d = dict(consts)
        d["w"] = np.ascontiguousarray(inp["w_in"][l][:, _wa_cols(hh)])
        d["dinT"], d["xiT"], d["zeta"], d["gch"] = _ret_tables(hh)
        s = slice(hh * 256, hh * 256 + 256)
        d["gng"] = rep128(inp["ret_gn_g"][l][s])
        d["gnb"] = rep128(inp["ret_gn_b"][l][s])
        d["lng"] = rep128(inp["sgu_ln_g"][l][s])
        d["lnb"] = rep128(inp["sgu_ln_b"][l][s])
        d["sguw"] = np.ascontiguousarray(inp["sgu_w"][l][2 * hh:2 * hh + 2])
        d["sgub"] = np.ascontiguousarray(inp["sgu_b"][l][2 * hh:2 * hh + 2].T)
        d["peT"] = np.ascontiguousarray(np.stack([inp["cmp_pe_k"][l].T, inp["cmp_pe_v"][l].T], 1))
        d["w1"] = np.ascontiguousarray(np.stack([inp["cmp_w1_k"][l], inp["cmp_w1_v"][l]], 0))
        d["w2"] = np.ascontiguousarray(np.stack([inp["cmp_w2_k"][l], inp["cmp_w2_v"][l]], 0))
        per_half[hh] = d
    for c in range(NCORES):
        b, hh = c // 2, c % 2
        d = dict(per_half[hh])
        d["x"] = np.ascontiguousarray(x[b])
        maps.append(d)
    return maps


def stage_a_gather(results):
    y = np.empty((BATCH, T, 2048), np.float32)
    for c in range(NCORES):
        b, hh = c // 2, c % 2
        yc = results[c]["y"]
        y[b, :, hh * 256:hh * 256 + 256] = yc[:, 0:256]
        y[b, :, 512 + hh * 256:512 + hh * 256 + 256] = yc[:, 256:512]
        y[b, :, 1024 + hh * 512:1024 + hh * 512 + 512] = yc[:, 512:1024]
    return y


TB = BATCH * SEQ // NCORES
NTB = TB // 128
NEXP = 16384
GS = 2


def layer_norm_tile(P, C, h, gam, bet, stt, out_f32, out_bf=None):
    st24, mv, sd, rstd = stt
    for q in range(4):
        P.op("dve", lambda e: e.bn_stats(out=st24[:, q * 6:(q + 1) * 6], in_=h[:, q * 512:(q + 1) * 512]), r=[h], w=[st24])
    P.op("dve", lambda e: e.bn_aggr(out=mv[:, :], in_=st24[:, :]), r=[st24], w=[mv])
    act(P, sd[:, :], mv[:, 1:2], AF.Sqrt, r=[mv, C.eps], w=[sd], bias=C.eps[:, 0:1])
    P.op("dve", lambda e: e.reciprocal(out=rstd[:, :], in_=sd[:, :]), r=[sd], w=[rstd])
    ts(P, "dve", out_f32[:, :], h[:, :], mv[:, 0:1], rstd[:, 0:1], ALU.subtract, ALU.mult, r=[h, mv, rstd], w=[out_f32])
    tt(P, "pool", out_f32[:, :], out_f32[:, :], gam[:, :], ALU.mult, r=[out_f32, gam], w=[out_f32])
    tt(P, "pool", out_f32[:, :], out_f32[:, :], bet[:, :], ALU.add, r=[out_f32, bet], w=[out_f32])
    if out_bf is not None:
        P.op("act", lambda e: e.copy(out=out_bf[:, :], in_=out_f32[:, :]), r=[out_f32], w=[out_bf])


def transpose_tile(P, C, src_bf, dstT, tps):
    for half in range(2):
        t = tps[half]
        for kk in range(8):
            k = half * 8 + kk
            tr(P, t[:, kk * 128:(kk + 1) * 128], src_bf[:, k * 128:(k + 1) * 128], C.identb[:, :], r=[src_bf, C.identb], w=[t])
        src = t[:, :].rearrange("p (k c) -> p k c", k=8)
        dst = dstT[:, half * 8:half * 8 + 8, :]
        if half == 0:
            P.op("dve", lambda e: e.tensor_copy(out=dst, in_=src), r=[t], w=[dstT])
        else:
            P.op("act", lambda e: e.copy(out=dst, in_=src), r=[t], w=[dstT])


def build_stage_b(phases="12"):
    nc = bass.Bass("TRN2", target_bir_lowering=False)
    es = ExitStack()
    with es:
        P = Prog(nc, es)
        C = Ctx()
        din = lambda n, s, dt=F32: P.dram(n, s, dt, "ExternalInput")
        C.y = din("y", [TB, 2048])
        C.xr = din("xr", [TB, 2048])
        C.wout = din("wout", [2048, 2048])
        C.wq = din("wq", [2048, 2048])
        C.mng = din("mng", [128, 2048])
        C.g1 = din("g1", [128, 2048])
        C.b1 = din("b1", [128, 2048])
        C.g2 = din("g2", [128, 2048])
        C.b2 = din("b2", [128, 2048])
        C.kT = din("kT", [128, 16, 128])
        C.u = din("u", [NEXP, 2048])
        C.v = din("v", [NEXP, 2048])
        C.ident = din("ident", [128, 128])
        C.iota16 = din("iota16", [128, 16])
        C.xo = P.dram("xo", [TB, 2048], F32, "ExternalOutput")
        C.x1d = P.dram("x1d", [TB, 2048], F32, "Internal")

        C.eps = P.sb("eps", [128, 1], F32)
        P.op("dve", lambda g: g.memset(C.eps[:, :], LN_EPS), w=[C.eps])
        C.identb = P.sb("identb", [128, 128], BF16)
        P.dma("pool", C.identb[:, :], C.ident[:, :], w=[C.identb])
        if "1" in phases:
            stage_b1(P, C)
        if "2" in phases:
            stage_b2(P, C)
        P.finish()
    return nc


def load_w2048(P, wbuf, src):
    for q in range(4):
        P.dma("pool", wbuf[:, :, q * 512:(q + 1) * 512],
              src[:, q * 512:(q + 1) * 512].rearrange("(k p) c -> p k c", p=128), w=[wbuf])


def stage_b1(P, C):
    P.push()
    wout = P.sb("wout", [128, 16, 2048], BF16)
    load_w2048(P, wout, C.wout)
    mng = P.sb("mng", [128, 2048], F32)
    g1 = P.sb("g1", [128, 2048], F32)
    b1 = P.sb("b1", [128, 2048], F32)
    P.dma("sp", mng[:, :], C.mng[:, :], w=[mng])
    P.dma("sp", g1[:, :], C.g1[:, :], w=[g1])
    P.dma("sp", b1[:, :], C.b1[:, :], w=[b1])
    yt = [P.sb("yt%d" % i, [128, 2048], F32) for i in range(2)]
    xt = [P.sb("xt%d" % i, [128, 2048], F32) for i in range(2)]
    h1 = [P.sb("h1_%d" % i, [128, 2048], F32) for i in range(2)]
    mb = P.sb("mb", [128, 2048], BF16)
    mT = P.sb("mT", [128, 16, 128], BF16)
    sq = P.sb("sqj", [128, 1024], BF16)
    ss = P.sb("ss", [128, 3], F32)
    rr = P.sb("rr", [128, 3], F32)
    stt = (P.sb("st24", [128, 24], F32), P.sb("mvb", [128, 2], F32), P.sb("sdb", [128, 1], F32), P.sb("rstdb", [128, 1], F32))
    tps = [P.ps("tpb%d" % i, [128, 1024], BF16) for i in range(2)]
    ops = [P.ps("opb%d" % i, [128, 512], F32) for i in range(4)]
    segs = ((0, 512), (512, 1024), (1024, 2048))
    for i in range(NTB):
        rs = slice(i * 128, (i + 1) * 128)
        y_, x_, h_ = yt[i % 2], xt[i % 2], h1[i % 2]
        P.dma("sp", y_[:, :], C.y[rs, :], w=[y_])
        P.dma("sp", x_[:, :], C.xr[rs, :], w=[x_])
        for j, (a, b) in enumerate(segs):
            act(P, sq[:, 0:b - a], y_[:, a:b], AF.Square, r=[y_], w=[sq, ss], accum=ss[:, j:j + 1])
        ts(P, "dve", rr[:, 0:2], ss[:, 0:2], 1.0 / 512, LN_EPS, ALU.mult, ALU.add, r=[ss], w=[rr])
        ts(P, "dve", rr[:, 2:3], ss[:, 2:3], 1.0 / 1024, LN_EPS, ALU.mult, ALU.add, r=[ss], w=[rr])
        act(P, rr[:, :], rr[:, :], AF.Sqrt, r=[rr], w=[rr])
        P.op("dve", lambda e: e.reciprocal(out=rr[:, :], in_=rr[:, :]), r=[rr], w=[rr])
        for j, (a, b) in enumerate(segs):
            P.op("dve", lambda e: e.scalar_tensor_tensor(out=mb[:, a:b], in0=y_[:, a:b], scalar=rr[:, j:j + 1], in1=mng[:, a:b],
                                                         op0=ALU.mult, op1=ALU.mult), r=[y_, rr, mng], w=[mb])
        transpose_tile(P, C, mb, mT, tps)
        for q in range(4):
            for k in range(16):
                mm(P, ops[q][:, :], mT[:, k, :], wout[:, k, q * 512:(q + 1) * 512], k == 0, k == 15, r=[mT, wout], w=[ops[q]])
            P.op("dve", lambda e: e.scalar_tensor_tensor(out=h_[:, q * 512:(q + 1) * 512], in0=x_[:, q * 512:(q + 1) * 512],
                                                         scalar=ALPHA, in1=ops[q][:, :], op0=ALU.mult, op1=ALU.add),
                 r=[x_, ops[q]], w=[h_])
        layer_norm_tile(P, C, h_, g1, b1, stt, h_)
        P.dma("sp", C.x1d[rs, :], h_[:, :], r=[h_], w=[C.x1d])
    P.pop()


def stage_b2(P, C):
    P.push()
    wq = P.sb("wq", [128, 16, 2048], BF16)
    load_w2048(P, wq, C.wq)
    g2 = P.sb("g2", [128, 2048], F32)
    b2 = P.sb("b2", [128, 2048], F32)
    P.dma("sp", g2[:, :], C.g2[:, :], w=[g2])
    P.dma("sp", b2[:, :], C.b2[:, :], w=[b2])
    kT = P.sb("kT", [128, 16, 128], BF16)
    P.dma("pool", kT[:, :, :], C.kT[:, :, :], w=[kT])
    iota16 = P.sb("iota16", [128, 16], F32)
    P.dma("sp", iota16[:, :], C.iota16[:, :], w=[iota16])
    x1 = P.sb("x1", [128, 2048], F32)
    x1b = P.sb("x1b", [128, 2048], BF16)
    x1T = P.sb("x1T", [128, 16, 128], BF16)
    qT = P.sb("qT", [128, 16, 128], BF16)
    s_sb = P.sb("s_sb", [128, 16, 128], F32)
    s_wk = P.sb("s_wk", [128, 128], F32)
    v16 = P.sb("v16", [128, 8, 2, 16], F32)
    i16 = P.sb("i16", [128, 8, 2, 16], U32)
    i16f = P.sb("i16f", [128, 8, 2, 16], F32)
    cand = P.sb("cand", [128, 8, 256], F32)
    c_wk = P.sb("c_wk", [128, 256], F32)
    sc16 = P.sb("sc16", [128, 8, 16], F32)
    ci = P.sb("ci", [128, 8, 16], U32)
    ca = P.sb("ca", [128, 8, 16], U32)
    cb = P.sb("cb", [128, 8, 16], U32)
    caf = P.sb("caf", [128, 8, 16], F32)
    cbf = P.sb("cbf", [128, 8, 16], F32)
    oh = P.sb("oh", [128, 8, 16, 16], F32)
    sel1 = P.sb("sel1", [128, 8, 16], F32)
    sel2 = P.sb("sel2", [128, 8, 16], F32)
    eidf = P.sb("eidf", [128, 128], F32)
    eid = P.sb("eid", [128, 128], I32)
    gw = P.sb("gw", [128, 8, 16], F32)
    gsum = P.sb("gsum", [128, 8], F32)
    hdot = P.sb("hdot", [128, 128], F32)
    gh = P.sb("gh", [128, 128], F32)
    junk = P.sb("junk", [128, 2048], BF16)
    acc = P.sb("acc", [128, 2048], F32)
    ue = [P.sb("ue%d" % i, [128, GS, 2048], F32) for i in range(2)]
    ve = ue
    stt = (P.sb("st24", [128, 24], F32), P.sb("mvb", [128, 2], F32), P.sb("sdb", [128, 1], F32), P.sb("rstdb", [128, 1], F32))
    tps = [P.ps("tpc%d" % i, [128, 1024], BF16) for i in range(2)]
    qps = [P.ps("qps%d" % i, [128, 512], F32) for i in range(2)]
    sps = [P.ps("sps%d" % i, [128, 512], F32) for i in range(4)]
    for i in range(NTB):
        rs = slice(i * 128, (i + 1) * 128)
        P.dma("sp", x1[:, :], C.x1d[rs, :], r=[C.x1d], w=[x1])
        P.op("act", lambda e: e.copy(out=x1b[:, :], in_=x1[:, :]), r=[x1], w=[x1b])
        transpose_tile(P, C, x1b, x1T, tps)
        for c4 in range(4):
            p = qps[c4 % 2]
            for cc in range(4):
                c = c4 * 4 + cc
                for k in range(16):
                    mm(P, p[:, cc * 128:(cc + 1) * 128], wq[:, k, c * 128:(c + 1) * 128], x1T[:, k, :], k == 0, k == 15,
                       r=[wq, x1T], w=[p])
            P.op("dve", lambda e: e.tensor_copy(out=qT[:, c4 * 4:c4 * 4 + 4, :], in_=p[:, :].rearrange("p (c t) -> p c t", c=4)),
                 r=[p], w=[qT])
        for c4 in range(4):
            p = sps[c4]
            for cc in range(4):
                c = c4 * 4 + cc
                mm(P, p[:, cc * 128:(cc + 1) * 128], qT[:, c, :], kT[:, c, :], True, True, r=[qT, kT], w=[p])
            P.op("act", lambda e: e.copy(out=s_sb[:, c4 * 4:c4 * 4 + 4, :], in_=p[:, :].rearrange("p (c t) -> p c t", c=4)),
                 r=[p], w=[s_sb])
        for c in range(16):
            h, hf = c // 2, c % 2
            P.op("dve", lambda e: e.max(out=v16[:, h, hf, 0:8], in_=s_sb[:, c, :]), r=[s_sb], w=[v16])
            P.op("dve", lambda e: e.max_index(out=i16[:, h, hf, 0:8], in_max=v16[:, h, hf, 0:8], in_values=s_sb[:, c, :]),
                 r=[s_sb, v16], w=[i16])
            P.op("dve", lambda e: e.match_replace(out=s_wk[:, :], in_to_replace=v16[:, h, hf, 0:8], in_values=s_sb[:, c, :],
                                                  imm_value=-1e30), r=[s_sb, v16], w=[s_wk])
            P.op("dve", lambda e: e.max(out=v16[:, h, hf, 8:16], in_=s_wk[:, :]), r=[s_wk], w=[v16])
            P.op("dve", lambda e: e.max_index(out=i16[:, h, hf, 8:16], in_max=v16[:, h, hf, 8:16], in_values=s_wk[:, :]),
                 r=[s_wk, v16], w=[i16])
        P.op("dve", lambda e: e.tensor_copy(out=i16f[:, :, :, :], in_=i16[:, :, :, :]), r=[i16], w=[i16f])
        tt(P, "dve", cand[:, :, :].rearrange("p h (a b) -> p h a b", a=16),
           v16[:, :, 0, :].unsqueeze(3).to_broadcast([128, 8, 16, 16]),
           v16[:, :, 1, :].unsqueeze(2).to_broadcast([128, 8, 16, 16]), ALU.add, r=[v16], w=[cand])
        for h in range(8):
            P.op("dve", lambda e: e.max(out=sc16[:, h, 0:8], in_=cand[:, h, :]), r=[cand], w=[sc16])
            P.op("dve", lambda e: e.max_index(out=ci[:, h, 0:8], in_max=sc16[:, h, 0:8], in_values=cand[:, h, :]),
                 r=[cand, sc16], w=[ci])
            P.op("dve", lambda e: e.match_replace(out=c_wk[:, :], in_to_replace=sc16[:, h, 0:8], in_values=cand[:, h, :],
                                                  imm_value=-1e30), r=[cand, sc16], w=[c_wk])
            P.op("dve", lambda e: e.max(out=sc16[:, h, 8:16], in_=c_wk[:, :]), r=[c_wk], w=[sc16])
            P.op("dve", lambda e: e.max_index(out=ci[:, h, 8:16], in_max=sc16[:, h, 8:16], in_values=c_wk[:, :]),
                 r=[c_wk, sc16], w=[ci])
        P.op("dve", lambda e: e.tensor_single_scalar(out=ca[:, :, :], in_=ci[:, :, :], scalar=4, op=ALU.logical_shift_right),
             r=[ci], w=[ca])
        P.op("dve", lambda e: e.tensor_single_scalar(out=cb[:, :, :], in_=ci[:, :, :], scalar=15, op=ALU.bitwise_and),
             r=[ci], w=[cb])
        P.op("dve", lambda e: e.tensor_copy(out=caf[:, :, :], in_=ca[:, :, :]), r=[ca], w=[caf])
        P.op("dve", lambda e: e.tensor_copy(out=cbf[:, :, :], in_=cb[:, :, :]), r=[cb], w=[cbf])
        for (sel, cf, hf) in ((sel1, caf, 0), (sel2, cbf, 1)):
            tt(P, "dve", oh[:, :, :, :].rearrange("p h j a -> p (h j) a"),
               cf[:, :, :].rearrange("p h j -> p (h j)").unsqueeze(2).to_broadcast([128, 128, 16]),
               iota16[:, :].unsqueeze(1).to_broadcast([128, 128, 16]), ALU.is_equal, r=[cf, iota16], w=[oh])
            tt(P, "dve", oh[:, :, :, :], oh[:, :, :, :], i16f[:, :, hf, :].unsqueeze(2).to_broadcast([128, 8, 16, 16]), ALU.mult,
               r=[oh, i16f], w=[oh])
            P.op("dve", lambda e: e.tensor_reduce(out=sel[:, :, :], in_=oh[:, :, :, :], axis=AX.X, op=ALU.add), r=[oh], w=[sel])
        P.op("dve", lambda e: e.scalar_tensor_tensor(out=eidf[:, :], in0=sel1[:, :, :].rearrange("p h j -> p (h j)"), scalar=128.0,
                                                     in1=sel2[:, :, :].rearrange("p h j -> p (h j)"), op0=ALU.mult, op1=ALU.add),
             r=[sel1, sel2], w=[eidf])
        P.op("dve", lambda e: e.tensor_copy(out=eid[:, :], in_=eidf[:, :]), r=[eidf], w=[eid])
        tt(P, "dve", gw[:, :, :], sc16[:, :, :], sc16[:, :, 0:1].to_broadcast([128, 8, 16]), ALU.subtract, r=[sc16], w=[gw])
        act(P, gw[:, :, :], gw[:, :, :], AF.Exp, r=[gw], w=[gw])
        P.op("dve", lambda e: e.tensor_reduce(out=gsum[:, :], in_=gw[:, :, :], axis=AX.X, op=ALU.add), r=[gw], w=[gsum])
        P.op("dve", lambda e: e.reciprocal(out=gsum[:, :], in_=gsum[:, :]), r=[gsum], w=[gsum])
        tt(P, "dve", gw[:, :, :], gw[:, :, :], gsum[:, :].unsqueeze(2).to_broadcast([128, 8, 16]), ALU.mult, r=[gw, gsum], w=[gw])
        ng = 128 // GS
        for g_ in range(ng):
            ub = ue[g_ % 2]
            for s in range(GS):
                sl = g_ * GS + s
                P.idma(ub[:, s, :], C.u[:, :], eid[:, sl:sl + 1], r=[eid], w=[ub])
            for s in range(GS):
                sl = g_ * GS + s
                P.op("dve", lambda e: e.scalar_tensor_tensor(out=junk[:, :], in0=ub[:, s, :], scalar=1.0, in1=x1[:, :],
                                                             op0=ALU.mult, op1=ALU.mult, accum_out=hdot[:, sl:sl + 1]),
                     r=[ub, x1], w=[junk, hdot])
        act(P, gh[:, :], hdot[:, :], AF.Gelu, r=[hdot], w=[gh])
        tt(P, "dve", gh[:, :], gh[:, :], gw[:, :, :].rearrange("p h j -> p (h j)"), ALU.mult, r=[gh, gw], w=[gh])
        for g_ in range(ng):
            vb = ve[g_ % 2]
            for s in range(GS):
                sl = g_ * GS + s
                P.idma(vb[:, s, :], C.v[:, :], eid[:, sl:sl + 1], r=[eid], w=[vb])
            for s in range(GS):
                sl = g_ * GS + s
                if sl == 0:
                    ts(P, "dve", acc[:, :], vb[:, s, :], gh[:, 0:1], None, ALU.mult, None, r=[vb, gh], w=[acc])
                else:
                    P.op("dve", lambda e: e.scalar_tensor_tensor(out=acc[:, :], in0=vb[:, s, :], scalar=gh[:, sl:sl + 1], in1=acc[:, :],
                                                                 op0=ALU.mult, op1=ALU.add), r=[vb, gh, acc], w=[acc])
        P.op("dve", lambda e: e.scalar_tensor_tensor(out=acc[:, :], in0=x1[:, :], scalar=ALPHA, in1=acc[:, :], op0=ALU.mult, op1=ALU.add),
             r=[x1, acc], w=[acc])
        layer_norm_tile(P, C, acc, g2, b2, stt, acc)
        P.dma("sp", C.xo[rs, :], acc[:, :], r=[acc])
    P.pop()


def stage_b_inputs(y, x, inp, l, ident):
    yf = y.reshape(BATCH * SEQ, 2048)
    xf = x.reshape(BATCH * SEQ, 2048)
    kT = np.empty((128, 16, 128), np.float32)
    for h in range(8):
        kT[:, 2 * h, :] = inp["peer_k1"][l][h].T
        kT[:, 2 * h + 1, :] = inp["peer_k2"][l][h].T
    shared = {
        "wout": np.ascontiguousarray(inp["w_out"][l]), "wq": np.ascontiguousarray(inp["peer_wq"][l]),
        "mng": rep128(inp["mix_norm_g"][l]), "g1": rep128(inp["ln1_g"][l]), "b1": rep128(inp["ln1_b"][l]),
        "g2": rep128(inp["ln2_g"][l]), "b2": rep128(inp["ln2_b"][l]), "kT": kT,
        "u": np.ascontiguousarray(inp["peer_u"][l]), "v": np.ascontiguousarray(inp["peer_v"][l]),
        "ident": ident, "iota16": rep128(np.arange(16, dtype=np.float32)),
    }
    maps = []
    for c in range(NCORES):
        d = dict(shared)
        d["y"] = np.ascontiguousarray(yf[c * TB:(c + 1) * TB])
        d["xr"] = np.ascontiguousarray(xf[c * TB:(c + 1) * TB])
        maps.append(d)
    return maps


_CACHE = {}


def kernel(**inputs):
    inp = {k: np.asarray(v) for k, v in inputs.items()}
    if "a" not in _CACHE:
        _CACHE["a"] = build_stage_a()
        _CACHE["b"] = build_stage_b()
        _CACHE["ca"] = _consts_a()
    consts = _CACHE["ca"]
    x = np.asarray(inp["x"], np.float32)
    cores = list(range(NCORES))
    for l in range(DEPTH):
        ra = run_bass_kernel_spmd(_CACHE["a"], stage_a_inputs(x, inp, l, consts), core_ids=cores)
        y = stage_a_gather(ra.results)
        rb = run_bass_kernel_spmd(_CACHE["b"], stage_b_inputs(y, x, inp, l, consts["ident"]), core_ids=cores)
        x = np.concatenate([rb.results[c]["xo"] for c in range(NCORES)], 0).reshape(BATCH, SEQ, D_MODEL)
    return x.astype(np.float32)
```
